# Optimizing a Trainium2 kernel written in Bass

```python
import math
import jax, jax.numpy as jnp
from jax import lax
import numpy as np

D_MODEL = 2048
BATCH = 1
SEQ = 8192
DEPTH = 4

FOX_HEAD_DIM = 128
FOX_HEADS = D_MODEL // 256
FOX_WIDTH = FOX_HEADS * FOX_HEAD_DIM
DIFF_QK_DIM = 64
DIFF_V_DIM = 2 * DIFF_QK_DIM
DIFF_HEADS = D_MODEL // 256
DIFF_QK_WIDTH = 2 * DIFF_HEADS * DIFF_QK_DIM
DIFF_WIDTH = DIFF_HEADS * DIFF_V_DIM
POOL_WINDOWS = (2, 4, 8, 16)
POOL_GROUPS = len(POOL_WINDOWS)
POOL_WIDTH = D_MODEL // 2
POOL_GROUP_DIM = POOL_WIDTH // POOL_GROUPS
N_BRANCHES = 3
BLOCK_Q = 128
ROPE_THETA = 10000.0
D_FF = 11 * D_MODEL // 4
N_EXPERTS = 8
TOP_K = 2
EXPERT_D_FF = D_FF
LN_EPS = 1e-5
DEEPNORM_ALPHA = (2 * DEPTH) ** 0.25
DEEPNORM_BETA = (8 * DEPTH) ** -0.25
N_DENSE = (DEPTH + 1) // 2
N_MOE = DEPTH // 2
IN_SPLITS = (FOX_WIDTH, FOX_WIDTH, FOX_WIDTH, DIFF_QK_WIDTH, DIFF_QK_WIDTH, DIFF_WIDTH, POOL_WIDTH, N_BRANCHES * D_MODEL, FOX_HEADS)
N_IN = sum(IN_SPLITS)

kernel_name = 'hybrid_fox_pool_diffattn_moe_deepnorm'


def layer_norm(x, g, b):
    xf = x.astype(jnp.float32)
    mu = jnp.mean(xf, axis=-1, keepdims=True)
    xc = xf - mu
    var = jnp.mean(xc * xc, axis=-1, keepdims=True)
    y = xc * lax.rsqrt(var + LN_EPS) * g.astype(jnp.float32) + b.astype(jnp.float32)
    return y.astype(x.dtype)


def rms_norm(x, g):
    xf = x.astype(jnp.float32)
    y = xf * lax.rsqrt(jnp.mean(xf * xf, axis=-1, keepdims=True) + LN_EPS) * g.astype(jnp.float32)
    return y.astype(x.dtype)


def rope_tables(seq):
    pos = jnp.arange(seq, dtype=jnp.float32)
    inv = ROPE_THETA ** (-jnp.arange(0, DIFF_QK_DIM, 2, dtype=jnp.float32) / DIFF_QK_DIM)
    ang = pos[:, None] * inv[None, :]
    return jnp.cos(ang), jnp.sin(ang)


def apply_rope(x, cos, sin):
    xf = x.astype(jnp.float32)
    x1, x2 = jnp.split(xf, 2, axis=-1)
    c = cos[None, :, None, :]
    s = sin[None, :, None, :]
    return jnp.concatenate([x1 * c - x2 * s, x2 * c + x1 * s], axis=-1).astype(x.dtype)


def causal_mask(start, seq):
    qpos = start + jnp.arange(BLOCK_Q)
    kpos = jnp.arange(seq)
    return kpos[None, :] <= qpos[:, None]


def split_query_blocks(t):
    b, s = t.shape[0], t.shape[1]
    t = t.reshape((b, s // BLOCK_Q, BLOCK_Q) + t.shape[2:])
    return jnp.moveaxis(t, 1, 0)


def merge_query_blocks(t):
    t = jnp.moveaxis(t, 0, 1)
    return t.reshape((t.shape[0], -1) + t.shape[3:])


def forgetting_attention(q, k, v, log_f):
    s, dh = q.shape[1], q.shape[3]
    c = jnp.cumsum(log_f, axis=1)
    c_k = jnp.transpose(c, (0, 2, 1))
    scale = dh ** -0.5
    starts = jnp.arange(s // BLOCK_Q) * BLOCK_Q

    def block(args):
        q_blk, c_blk, start = args
        logits = jnp.einsum('bqhd,bkhd->bhqk', q_blk, k, preferred_element_type=jnp.float32) * scale
        logits = logits + jnp.transpose(c_blk, (0, 2, 1))[..., None] - c_k[:, :, None, :]
        logits = jnp.where(causal_mask(start, s), logits, -jnp.inf)
        p = jax.nn.softmax(logits, axis=-1)
        return jnp.einsum('bhqk,bkhd->bqhd', p.astype(v.dtype), v)

    out = lax.map(block, (split_query_blocks(q), split_query_blocks(c), starts))
    return merge_query_blocks(out)


def differential_attention(q1, q2, k1, k2, v, lam):
    s, dqk = q1.shape[1], q1.shape[3]
    scale = dqk ** -0.5
    starts = jnp.arange(s // BLOCK_Q) * BLOCK_Q

    def block(args):
        q1_blk, q2_blk, start = args
        mask = causal_mask(start, s)
        l1 = jnp.einsum('bqhd,bkhd->bhqk', q1_blk, k1, preferred_element_type=jnp.float32) * scale
        l2 = jnp.einsum('bqhd,bkhd->bhqk', q2_blk, k2, preferred_element_type=jnp.float32) * scale
        p1 = jax.nn.softmax(jnp.where(mask, l1, -jnp.inf), axis=-1)
        p2 = jax.nn.softmax(jnp.where(mask, l2, -jnp.inf), axis=-1)
        p = p1 - lam * p2
        return jnp.einsum('bhqk,bkhd->bqhd', p.astype(v.dtype), v)

    out = lax.map(block, (split_query_blocks(q1), split_query_blocks(q2), starts))
    return merge_query_blocks(out)


def multiscale_pool(u, w_group, scale):
    b, s = u.shape[0], u.shape[1]
    uf = u.astype(jnp.float32)
    csum = jnp.cumsum(uf, axis=1)
    t = jnp.arange(1, s + 1, dtype=jnp.float32)
    pooled = []
    for g, w in enumerate(POOL_WINDOWS):
        cg = csum[:, :, g]
        shifted = jnp.pad(cg, ((0, 0), (w, 0), (0, 0)))[:, :s]
        count = jnp.minimum(t, float(w))[None, :, None]
        pooled.append((cg - shifted) / count)
    pooled = jnp.stack(pooled, axis=2)
    delta = (pooled - uf).astype(u.dtype)
    y = jnp.einsum('bsgc,gcd->bsgd', delta, w_group) * scale.reshape(POOL_GROUPS, POOL_GROUP_DIM)
    return y.reshape(b, s, POOL_WIDTH)


def hybrid_mixer(x, w_in, b_forget, w_pool_group, pool_scale, diff_lambda, diff_norm_gain,
                 w_branch_a, w_branch_b, w_branch_c, b_gate, w_out, lambda_init, cos, sin):
    b, s, _ = x.shape
    offsets = [int(o) for o in np.cumsum(IN_SPLITS)[:-1]]
    proj = jnp.einsum('bsd,dn->bsn', x, w_in)
    fq, fk, fv, dq, dk, dv, pu, gates, fg = jnp.split(proj, offsets, axis=-1)

    log_f = jax.nn.log_sigmoid(fg.astype(jnp.float32) + b_forget.astype(jnp.float32))
    y_a = forgetting_attention(fq.reshape(b, s, FOX_HEADS, FOX_HEAD_DIM),
                               fk.reshape(b, s, FOX_HEADS, FOX_HEAD_DIM),
                               fv.reshape(b, s, FOX_HEADS, FOX_HEAD_DIM), log_f)
    y_a = y_a.reshape(b, s, FOX_WIDTH)

    y_b = multiscale_pool(pu.reshape(b, s, POOL_GROUPS, POOL_GROUP_DIM), w_pool_group, pool_scale)

    dq = apply_rope(dq.reshape(b, s, 2 * DIFF_HEADS, DIFF_QK_DIM), cos, sin).reshape(b, s, DIFF_HEADS, 2, DIFF_QK_DIM)
    dk = apply_rope(dk.reshape(b, s, 2 * DIFF_HEADS, DIFF_QK_DIM), cos, sin).reshape(b, s, DIFF_HEADS, 2, DIFF_QK_DIM)
    lp = diff_lambda.astype(jnp.float32)
    lam = jnp.exp(jnp.sum(lp[0] * lp[1])) - jnp.exp(jnp.sum(lp[2] * lp[3])) + lambda_init
    o = differential_attention(dq[:, :, :, 0], dq[:, :, :, 1], dk[:, :, :, 0], dk[:, :, :, 1],
                               dv.reshape(b, s, DIFF_HEADS, DIFF_V_DIM), lam)
    o = rms_norm(o, diff_norm_gain) * (1.0 - lambda_init)
    y_c = o.reshape(b, s, DIFF_WIDTH)

    g = jax.nn.sigmoid(gates.reshape(b, s, N_BRANCHES, D_MODEL).astype(jnp.float32)
                       + b_gate.astype(jnp.float32)).astype(x.dtype)
    h = (g[:, :, 0] * (y_a @ w_branch_a)
         + g[:, :, 1] * (y_b @ w_branch_b)
         + g[:, :, 2] * (y_c @ w_branch_c))
    return h @ w_out


def swiglu(x, w_gate, w_up, w_down):
    return (jax.nn.silu(x @ w_gate) * (x @ w_up)) @ w_down


def moe_swiglu(x, router_w, router_b, w_gate, w_up, w_down):
    logits = jnp.einsum('bsd,de->bse', x, router_w, preferred_element_type=jnp.float32) + router_b.astype(jnp.float32)
    top_v, top_i = lax.top_k(logits, TOP_K)
    top_w = jax.nn.softmax(top_v, axis=-1)
    combine = jnp.sum(jax.nn.one_hot(top_i, N_EXPERTS, dtype=jnp.float32) * top_w[..., None], axis=-2)
    y = jnp.zeros(x.shape, jnp.float32)
    for e in range(N_EXPERTS):
        y = y + combine[..., e:e + 1] * swiglu(x, w_gate[e], w_up[e], w_down[e]).astype(jnp.float32)
    return y.astype(x.dtype)


def setup_inputs(seed: int = 0) -> dict:
    key = jax.random.key(seed)
    ks = jax.random.split(key, 24)
    f32 = jnp.float32
    beta = DEEPNORM_BETA

    def nrm(k, shape, scale):
        return jax.random.normal(k, shape, f32) * scale

    col_scales = (1.0, 1.0, beta, 1.0, 1.0, beta, 1.0, 1.0, 1.0)
    col_scale = jnp.asarray(np.concatenate([np.full(n, sc, np.float32) for n, sc in zip(IN_SPLITS, col_scales)]))
    return {
        'x': nrm(ks[0], (BATCH, SEQ, D_MODEL), 1.0),
        'w_in': nrm(ks[1], (DEPTH, D_MODEL, N_IN), D_MODEL ** -0.5) * col_scale,
        'b_forget': jax.random.uniform(ks[2], (DEPTH, FOX_HEADS), f32, 1.0, 5.0),
        'w_pool_group': nrm(ks[3], (DEPTH, POOL_GROUPS, POOL_GROUP_DIM, POOL_GROUP_DIM), POOL_GROUP_DIM ** -0.5),
        'pool_scale': 1.0 + nrm(ks[4], (DEPTH, POOL_WIDTH), 0.1),
        'diff_lambda': nrm(ks[5], (DEPTH, 4, DIFF_QK_DIM), 0.1),
        'diff_norm_gain': 1.0 + nrm(ks[6], (DEPTH, DIFF_V_DIM), 0.02),
        'w_branch_a': nrm(ks[7], (DEPTH, FOX_WIDTH, D_MODEL), FOX_WIDTH ** -0.5 * beta),
        'w_branch_b': nrm(ks[8], (DEPTH, POOL_WIDTH, D_MODEL), POOL_WIDTH ** -0.5 * beta),
        'w_branch_c': nrm(ks[9], (DEPTH, DIFF_WIDTH, D_MODEL), DIFF_WIDTH ** -0.5 * beta),
        'b_gate': nrm(ks[10], (DEPTH, N_BRANCHES, D_MODEL), 0.02),
        'w_out': nrm(ks[11], (DEPTH, D_MODEL, D_MODEL), D_MODEL ** -0.5 * beta),
        'ln1_g': 1.0 + nrm(ks[12], (DEPTH, D_MODEL), 0.02),
        'ln1_b': nrm(ks[13], (DEPTH, D_MODEL), 0.02),
        'ln2_g': 1.0 + nrm(ks[14], (DEPTH, D_MODEL), 0.02),
        'ln2_b': nrm(ks[15], (DEPTH, D_MODEL), 0.02),
        'ffn_w_gate': nrm(ks[16], (N_DENSE, D_MODEL, D_FF), D_MODEL ** -0.5),
        'ffn_w_up': nrm(ks[17], (N_DENSE, D_MODEL, D_FF), D_MODEL ** -0.5 * beta),
        'ffn_w_down': nrm(ks[18], (N_DENSE, D_FF, D_MODEL), D_FF ** -0.5 * beta),
        'router_w': nrm(ks[19], (N_MOE, D_MODEL, N_EXPERTS), D_MODEL ** -0.5),
        'router_b': nrm(ks[20], (N_MOE, N_EXPERTS), 0.01),
        'expert_w_gate': nrm(ks[21], (N_MOE, N_EXPERTS, D_MODEL, EXPERT_D_FF), D_MODEL ** -0.5),
        'expert_w_up': nrm(ks[22], (N_MOE, N_EXPERTS, D_MODEL, EXPERT_D_FF), D_MODEL ** -0.5 * beta),
        'expert_w_down': nrm(ks[23], (N_MOE, N_EXPERTS, EXPERT_D_FF, D_MODEL), EXPERT_D_FF ** -0.5 * beta),
    }


def reference(x, w_in, b_forget, w_pool_group, pool_scale, diff_lambda, diff_norm_gain,
              w_branch_a, w_branch_b, w_branch_c, b_gate, w_out, ln1_g, ln1_b, ln2_g, ln2_b,
              ffn_w_gate, ffn_w_up, ffn_w_down, router_w, router_b,
              expert_w_gate, expert_w_up, expert_w_down):
    cos, sin = rope_tables(x.shape[1])
    for layer in range(DEPTH):
        lambda_init = 0.8 - 0.6 * math.exp(-0.3 * layer)
        mix = hybrid_mixer(x, w_in[layer], b_forget[layer], w_pool_group[layer], pool_scale[layer],
                           diff_lambda[layer], diff_norm_gain[layer], w_branch_a[layer], w_branch_b[layer],
                           w_branch_c[layer], b_gate[layer], w_out[layer], lambda_init, cos, sin)
        x = layer_norm(DEEPNORM_ALPHA * x + mix, ln1_g[layer], ln1_b[layer])
        j = layer // 2
        if layer % 2 == 0:
            f = swiglu(x, ffn_w_gate[j], ffn_w_up[j], ffn_w_down[j])
        else:
            f = moe_swiglu(x, router_w[j], router_b[j], expert_w_gate[j], expert_w_up[j], expert_w_down[j])
        x = layer_norm(DEEPNORM_ALPHA * x + f, ln2_g[layer], ln2_b[layer])
    return x
```

```python
import numpy as np
import concourse.bass as bass
import concourse.mybir as mybir
from concourse.bass_utils import run_bass_kernel_spmd
from contextlib import ExitStack

F32 = mybir.dt.float32
BF16 = mybir.dt.bfloat16
AF = mybir.ActivationFunctionType
ALU = mybir.AluOpType
AX = mybir.AxisListType


class _Op:
    __slots__ = ("eng", "fn", "deps", "dma", "signal", "tok", "inc", "psem", "extw")

    def __init__(self, eng, fn, deps, dma):
        self.eng = eng
        self.fn = fn
        self.deps = deps
        self.dma = dma
        self.signal = False
        self.tok = None
        self.inc = 16 if dma is not None else 1
        self.psem = None
        self.extw = None


class Prog:
    ENGS = ("pe", "act", "dve", "pool", "sp")
    COLL_INC = 1
    NPROG = 0

    def __init__(self, nc, es):
        self.nc = nc
        self.es = es
        self.ops = []
        self.last_w = {}
        self.readers = {}

    def _deps(self, reads, writes):
        deps = []
        for b in reads:
            w = self.last_w.get(b)
            if w is not None:
                deps.append(w)
        for b in writes:
            w = self.last_w.get(b)
            if w is not None:
                deps.append(w)
            deps.extend(self.readers.get(b, ()))
        return deps

    def _record(self, op, reads, writes):
        self.ops.append(op)
        for b in reads:
            self.readers.setdefault(b, []).append(op)
        for b in writes:
            self.last_w[b] = op
            self.readers[b] = []

    def op(self, eng, fn, reads=(), writes=()):
        o = _Op(eng, fn, self._deps(reads, writes), None)
        self._record(o, reads, writes)
        return o

    def dma(self, eng, out, in_, reads=(), writes=(), chan=None, **kw):
        assert chan is not None
        o = _Op(eng, lambda e: e.dma_start(out=out, in_=in_, **kw), self._deps(reads, writes), chan)
        self._record(o, reads, writes)
        return o

    def coll(self, kind, alu, in_, out, reads=(), writes=(), chan=None):
        rg = [list(range(8))]
        o = _Op("pool", lambda e: e.collective_compute(kind, alu, replica_groups=rg, ins=[in_.opt()], outs=[out.opt()]),
                self._deps(reads, writes), chan)
        o.inc = self.COLL_INC
        self._record(o, reads, writes)
        return o

    def coll_persist(self, kind, alu, in_, out, sem):
        rg = [list(range(8))]
        o = _Op("pool", lambda e: e.collective_compute(kind, alu, replica_groups=rg, ins=[in_.opt()], outs=[out.opt()]), [], None)
        o.psem = sem
        self.ops.append(o)
        return o

    def ext_wait(self, eng, sem, val):
        o = _Op(eng, None, [], None)
        o.extw = [(sem, val)]
        self.ops.append(o)
        return o

    def wait(self, eng, bufs):
        o = _Op(eng, None, self._deps(bufs, ()), None)
        self._record(o, bufs, ())
        return o

    def emit(self):
        nc, es = self.nc, self.es
        for o in self.ops:
            if o.dma is not None:
                o.signal = True
            for d in o.deps:
                if not (d.eng == "pe" and o.eng == "pe" and d.dma is None and o.dma is None):
                    d.signal = True
        cnt = {}
        sems = {}
        gen = {}
        LIM = 30000
        for o in self.ops:
            if o.fn is None or not o.signal:
                continue
            base = ("dma", o.dma) if o.dma is not None else o.eng
            inc = o.inc
            g = gen.get(base, 0)
            if cnt.get((base, g), 0) + inc > LIM:
                g += 1
                gen[base] = g
            key = (base, g)
            if key not in sems:
                sems[key] = nc.alloc_semaphore("s%d_%d" % (Prog.NPROG, len(sems)))
            cnt[key] = cnt.get(key, 0) + inc
            o.tok = (key, cnt[key])
        streams = {e: [] for e in self.ENGS}
        seen = {e: {} for e in self.ENGS}
        for o in self.ops:
            need = {}
            for d in o.deps:
                if d.tok is None:
                    continue
                if d.eng == "pe" and o.eng == "pe" and d.dma is None and o.dma is None:
                    continue
                k, v = d.tok
                if seen[o.eng].get(k, 0) >= v:
                    continue
                if need.get(k, 0) < v:
                    need[k] = v
            for k, v in need.items():
                seen[o.eng][k] = v
            streams[o.eng].append((o, [(sems[k], v) for k, v in need.items()] + (o.extw or [])))
        self.n_instr = {e: len(s) for e, s in streams.items()}
        fin = _Op("sp", None, [], None)
        fw = []
        for key, v in cnt.items():
            if isinstance(key[0], tuple) and gen.get(key[0], 0) == key[1] and seen["sp"].get(key, 0) < v:
                fw.append((sems[key], v))
        streams["sp"].append((fin, fw))
        Prog.NPROG += 1

        def run(eng, lst):
            for o, waits in lst:
                for s, v in waits:
                    eng.wait_ge(s, v)
                if o.fn is None:
                    continue
                ins = o.fn(eng)
                if o.psem is not None:
                    ins.then_inc(o.psem, 1)
                if o.tok is not None:
                    ins.then_inc(sems[o.tok[0]], o.inc)

        with nc.Block() as block:
            @block.tensor
            def _(e):
                run(e, streams["pe"])

            @block.scalar
            def _(e):
                run(e, streams["act"])

            @block.vector
            def _(e):
                run(e, streams["dve"])

            @block.gpsimd
            def _(e):
                run(e, streams["pool"])

            @block.sync
            def _(e):
                run(e, streams["sp"])

        nc.all_engine_barrier()
        if sems:
            nc.clear_and_free_semaphores(list(sems.values()))
            nc.all_engine_barrier()


D = 2048
S = 8192
NC = 8
TL = S // NC
KC = D // 128
TT = 512
NTT = S // TT
DEPTH = 4
DFF = 5632
NFF = DFF // 128
NEXP = 8
ALPHA = (2 * DEPTH) ** 0.25
LN_EPS = 1e-5
NEG = -30000.0
NHB = 5
POOL_W = (2, 4, 8, 16)


def lam_init(layer):
    import math
    return 0.8 - 0.6 * math.exp(-0.3 * layer)


_UN = [0]


def uname(name):
    _UN[0] += 1
    return "%s_%d" % (name, _UN[0])


class Ctx:
    pass


def chunked(ap2d, p=128):
    return ap2d.rearrange("(c p) n -> p c n", p=p)


def phase_a1(nc, cx, layer, mode, x_src, x_cast, w_head_l, yt_out):
    with ExitStack() as es:
        P = Prog(nc, es)
        sb = lambda name, shape, dt: es.enter_context(nc.sbuf_tensor(uname(name), shape, dt))
        W = sb("a1_w", [128, KC, NHB * 128], BF16)
        xb = [sb("a1_x%d" % i, [128, KC, TT], BF16) for i in range(2)]
        cs = [sb("a1_c%d" % i, [128, TT], F32) for i in range(2)]
        sn = [sb("a1_s%d" % i, [128, TT], F32) for i in range(2)]
        t1 = [sb("a1_t1%d" % i, [128, TT], F32) for i in range(2)]
        t2 = [sb("a1_t2%d" % i, [128, TT], F32) for i in range(2)]
        ub = [sb("a1_u%d" % i, [128, 16 + TT], F32) for i in range(2)]
        s2 = sb("a1_s2", [128, 16 + TT], F32)
        s4 = sb("a1_s4", [128, 16 + TT], F32)
        s8 = sb("a1_s8", [128, 16 + TT], F32)
        s16 = sb("a1_s16", [128, TT], F32)
        pacc = sb("a1_pacc", [128, TT], F32)
        dlt = [sb("a1_dl%d" % i, [128, TT], BF16) for i in range(2)]
        ee = [sb("a1_e%d" % i, [128, TT], F32) for i in range(2)]
        ll = [sb("a1_l%d" % i, [128, TT], F32) for i in range(2)]
        dg = sb("a1_dg", [128, 128], F32)
        ps = cx.ps

        wv = chunked(w_head_l)
        for q in range(4):
            P.dma("pool", W[:, q * 4:(q + 1) * 4, :], wv[:, q * 4:(q + 1) * 4, :],
                  writes=[("W", q)], chan=("a1w", q))
        P.op("dve", lambda e: e.tensor_scalar(cx.negb[:], cx.bfg[:, layer:layer + 1], -1.0, None, ALU.mult),
             reads=["consts"], writes=["negb"])
        P.op("pool", lambda e: e.memset(ub[0][:, 0:16], 0.0), writes=[("ubh", 0)])

        bank = 0
        def do_tile(t):
            nonlocal bank
            r, half = t // 2, t % 2
            xt = xb[t % 2]
            tok = slice(t * TT, (t + 1) * TT)
            xs = x_src[r].rearrange("(c p) n -> p c n", p=128)[:, :, half * TT:(half + 1) * TT]
            P.dma("pool" if x_cast else "sp", xt[:], xs, writes=[("x", t % 2)], chan=("a1x", t % 2))
            P.dma("sp", cs[t % 2][:], cx.ropeC[:, tok], writes=[("cs", t % 2)], chan=("a1c", t % 2))
            P.dma("sp", sn[t % 2][:], cx.ropeS[:, tok], writes=[("sn", t % 2)], chan=("a1s", t % 2))

            def proj(o):
                nonlocal bank
                b = bank
                bank = (bank + 1) % 6
                for kc in range(KC):
                    P.op("pe", lambda e, b=b, kc=kc, o=o, xt=xt: e.matmul(
                        ps[b][:], W[:, kc, o * 128:(o + 1) * 128], xt[:, kc, :], start=(kc == 0), stop=(kc == KC - 1)),
                        reads=[("W", kc // 4), ("x", t % 2)], writes=[("ps", b)])
                return b

            if mode == 'F':
                b = proj(0)
                P.op("act", lambda e, b=b: e.activation(cx.QT[:, tok], ps[b][:], AF.Copy, scale=128 ** -0.5),
                     reads=[("ps", b)], writes=[("QT", t)])
                b = proj(1)
                P.op("act", lambda e, b=b: e.activation(cx.KT[:, tok], ps[b][:], AF.Copy),
                     reads=[("ps", b)], writes=[("KT", t)])
                b = proj(2)
                u = ub[t % 2]
                up = ub[(t + 1) % 2]
                P.op("act", lambda e, b=b, u=u: e.activation(u[:, 16:16 + TT], ps[b][:], AF.Copy),
                     reads=[("ps", b)], writes=[("ub", t % 2)])
                if t > 0:
                    P.op("pool", lambda e, u=u, up=up: e.tensor_copy(u[:, 0:16], up[:, TT:TT + 16]),
                         reads=[("ub", (t + 1) % 2)], writes=[("ubh", t % 2)])
                rd = [("ub", t % 2), ("ubh", t % 2)]
                P.op("pool", lambda e, u=u: e.tensor_tensor(s2[:, 1:], u[:, 1:], u[:, 0:15 + TT], ALU.add),
                     reads=rd, writes=["s2"])
                P.op("pool", lambda e: e.tensor_tensor(s4[:, 3:], s2[:, 3:], s2[:, 1:14 + TT], ALU.add),
                     reads=["s2"], writes=["s4"])
                P.op("pool", lambda e: e.tensor_tensor(s8[:, 7:], s4[:, 7:], s4[:, 3:12 + TT], ALU.add),
                     reads=["s4"], writes=["s8"])
                P.op("pool", lambda e: e.tensor_tensor(s16[:], s8[:, 16:], s8[:, 8:8 + TT], ALU.add),
                     reads=["s8"], writes=["s16"])
                P.op("pool", lambda e: e.tensor_scalar(pacc[:], s2[:, 16:], cx.pcoef[:, 0:1], None, ALU.mult),
                     reads=["s2", "consts"], writes=["pacc"])
                for g, sg in ((1, s4), (2, s8)):
                    P.op("dve", lambda e, g=g, sg=sg: e.scalar_tensor_tensor(
                        pacc[:], sg[:, 16:], cx.pcoef[:, g:g + 1], pacc[:], ALU.mult, ALU.add),
                        reads=["s4", "s8", "pacc"], writes=["pacc"])
                P.op("dve", lambda e: e.scalar_tensor_tensor(
                    pacc[:], s16[:], cx.pcoef[:, 3:4], pacc[:], ALU.mult, ALU.add),
                    reads=["s16", "pacc"], writes=["pacc"])
                if t == 0:
                    P.op("pool", lambda e: e.tensor_tensor(pacc[:, 0:16], pacc[:, 0:16], cx.pcorr[:, 0:16], ALU.mult),
                         reads=["pacc", "consts"], writes=["pacc"])
                d = dlt[t % 2]
                P.op("pool", lambda e, d=d, u=u: e.tensor_tensor(d[:], pacc[:], u[:, 16:16 + TT], ALU.subtract),
                     reads=["pacc", ("ub", t % 2)], writes=[("dlt", t % 2)])
                P.dma("sp", yt_out[256:384, tok], d[:], reads=[("dlt", t % 2)], writes=["yt_out"], chan=("a1d", t % 2))
                b = proj(3)
                i = t % 2
                P.op("act", lambda e, b=b, i=i: e.activation(ee[i][:], ps[b][:], AF.Exp, bias=cx.negb[:], scale=-1.0),
                     reads=[("ps", b), "negb"], writes=[("ee", i)])
                P.op("act", lambda e, i=i: e.activation(ll[i][:], ee[i][:], AF.Ln, bias=1.0, scale=1.0),
                     reads=[("ee", i)], writes=[("ll", i)])
                init = 0.0 if t == 0 else cx.Lc[:, t * TT - 1:t * TT]
                P.op("dve", lambda e, i=i, init=init: e.tensor_tensor_scan(
                    cx.Lc[:, tok], ll[i][:], ll[i][:], init, ALU.add, ALU.max),
                    reads=[("ll", i), ("Lc", t - 1)], writes=[("Lc", t)])
                for j in range(4):
                    kb = t * 4 + j
                    P.op("dve", lambda e, kb=kb: e.tensor_tensor(dg[:], cx.Lc[:, kb * 128:(kb + 1) * 128], cx.ident[:], ALU.mult),
                         reads=[("Lc", t), "consts"], writes=["dg"])
                    P.op("dve", lambda e, kb=kb: e.reduce_sum(cx.ckcol[:, kb:kb + 1], dg[:], AX.X),
                         reads=["dg"], writes=[("ckcol", kb)])
            else:
                for (bo, bs, dst, nm) in ((0, 2, cx.QT, "QT"), (1, 3, cx.KT, "KT")):
                    b1 = proj(bo)
                    b2 = proj(bs)
                    i = t % 2
                    P.op("dve", lambda e, b1=b1, i=i: e.tensor_tensor(t1[i][:], ps[b1][:], cs[i][:], ALU.mult),
                         reads=[("ps", b1), ("cs", i)], writes=[("t1", i)])
                    P.op("dve", lambda e, b2=b2, i=i: e.tensor_tensor(t2[i][:], ps[b2][:], sn[i][:], ALU.mult),
                         reads=[("ps", b2), ("sn", i)], writes=[("t2", i)])
                    P.op("pool", lambda e, i=i, dst=dst: e.tensor_tensor(dst[:, tok], t1[i][:], t2[i][:], ALU.add),
                         reads=[("t1", i), ("t2", i)], writes=[(nm, t)])
            for j in range(4):
                kb = t * 4 + j
                b = 6 + (j % 2)
                for kc in range(KC):
                    P.op("pe", lambda e, b=b, kc=kc, j=j, xt=xt: e.matmul(
                        ps[b][:, 0:128], xt[:, kc, j * 128:(j + 1) * 128], W[:, kc, 4 * 128:5 * 128],
                        start=(kc == 0), stop=(kc == KC - 1)),
                        reads=[("W", kc // 4), ("x", t % 2)], writes=[("ps", b)])
                P.op("act", lambda e, b=b, kb=kb: e.activation(cx.V[:, kb, :], ps[b][:, 0:128], AF.Copy),
                     reads=[("ps", b)], writes=[("V", kb)])

        for t in range(NTT):
            do_tile(t)
        if getattr(cx, "dbg", None) is not None and mode == 'F':
            allk = lambda nm, n: [(nm, i) for i in range(n)]
            P.dma("sp", cx.dbg["QT"], cx.QT[:], reads=allk("QT", NTT), writes=["dbg1"], chan="dbg1")
            P.dma("sp", cx.dbg["KT"], cx.KT[:], reads=allk("KT", NTT), writes=["dbg2"], chan="dbg2")
            P.dma("sp", cx.dbg["V"], cx.V[:], reads=allk("V", 64), writes=["dbg3"], chan="dbg3")
            P.dma("sp", cx.dbg["Lc"], cx.Lc[:], reads=allk("Lc", NTT), writes=["dbg4"], chan="dbg4")
            P.dma("sp", cx.dbg["ck"], cx.ckcol[:], reads=allk("ckcol", 64), writes=["dbg5"], chan="dbg5")
        P.wait("sp", ["yt_out"])
        P.emit()
    return P


def phase_a2_fox(nc, cx, layer, yt_out):
    ps = cx.ps
    with ExitStack() as es:
        P = Prog(nc, es)
        sb = lambda name, shape, dt: es.enter_context(nc.sbuf_tensor(uname(name), shape, dt))
        NB = 3
        tmp = [sb("f_tmp%d" % i, [128, TT], F32) for i in range(NB)]
        pt = [sb("f_pt%d" % i, [128, TT], BF16) for i in range(NB)]
        nlq = [sb("f_nlq%d" % i, [128, TT], F32) for i in range(4)]
        rinv = sb("f_rinv", [128, TT], F32)
        yo = [sb("f_yo%d" % i, [128, TT], BF16) for i in range(2)]
        it = 0
        def do_qt_f(qt):
            nonlocal it
            qs = slice(qt * TT, (qt + 1) * TT)
            po, pr = 4 + 2 * (qt % 2), 5 + 2 * (qt % 2)
            nkb = 4 * qt + 4
            for o in range(4):
                P.op("pool", lambda e, o=o: e.tensor_tensor(nlq[o][:], cx.maskadd[:, o, :], cx.Lc[:, qs], ALU.subtract),
                     reads=["consts"], writes=[("nlq", o)])
            for kb in range(nkb):
                i = it % NB
                b = it % 4
                it += 1
                P.op("pe", lambda e, b=b, kb=kb: e.matmul(ps[b][:], cx.KT[:, kb * 128:(kb + 1) * 128], cx.QT[:, qs],
                                                           start=True, stop=True), writes=[("ps", b)])
                o = kb - 4 * qt
                if o < 0:
                    P.op("dve", lambda e, b=b, i=i: e.tensor_tensor(tmp[i][:], ps[b][:], cx.Lc[:, qs], ALU.subtract),
                         reads=[("ps", b)], writes=[("tmp", i)])
                else:
                    P.op("dve", lambda e, b=b, i=i, o=o: e.tensor_tensor(tmp[i][:], ps[b][:], nlq[o][:], ALU.add),
                         reads=[("ps", b), ("nlq", o)], writes=[("tmp", i)])
                P.op("act", lambda e, i=i, kb=kb: e.activation(pt[i][:], tmp[i][:], AF.Exp, bias=cx.ckcol[:, kb:kb + 1]),
                     reads=[("tmp", i)], writes=[("pt", i)])
                P.op("pe", lambda e, i=i, kb=kb: e.matmul(ps[po][:], cx.V[:, kb, :], pt[i][:], start=(kb == 0), stop=(kb == nkb - 1)),
                     reads=[("pt", i)], writes=[("ps", po)])
                P.op("pe", lambda e, i=i, kb=kb: e.matmul(ps[pr][:], cx.ones_bf[:], pt[i][:], start=(kb == 0), stop=(kb == nkb - 1)),
                     reads=[("pt", i)], writes=[("ps", pr)])
            y = yo[qt % 2]
            P.op("dve", lambda e: e.reciprocal(rinv[:], ps[pr][:]), reads=[("ps", pr)], writes=["rinv"])
            P.op("dve", lambda e, y=y: e.tensor_tensor(y[:], ps[po][:], rinv[:], ALU.mult),
                 reads=[("ps", po), "rinv"], writes=[("yo", qt % 2)])
            P.dma("sp", yt_out[0:128, qs], y[:], reads=[("yo", qt % 2)], writes=["yt_out"], chan=("fy", qt % 2))

        for qt in range(NTT):
            do_qt_f(qt)
        P.wait("sp", ["yt_out"])
        P.emit()


def phase_a2_diff(nc, cx, layer, yt_out):
    ps = cx.ps
    with ExitStack() as es:
        P = Prog(nc, es)
        sb = lambda name, shape, dt: es.enter_context(nc.sbuf_tensor(uname(name), shape, dt))
        NB = 2
        tmp = [[sb("d_tmp%d%d" % (m, i), [128, TT], F32) for i in range(NB)] for m in range(2)]
        pt = [[sb("d_pt%d%d" % (m, i), [128, TT], BF16) for i in range(NB)] for m in range(2)]
        rinv = sb("d_rinv", [128, TT], F32)
        oa = sb("d_oa", [128, TT], F32)
        ob = sb("d_ob", [128, TT], F32)
        oo = sb("d_oo", [128, TT], F32)
        sq = sb("d_sq", [128, TT], F32)
        lnv = sb("d_lnv", [128, TT], F32)
        rstd = sb("d_rstd", [128, TT], F32)
        yo = [sb("d_yo%d" % i, [128, TT], BF16) for i in range(2)]
        lt = sb("d_lt", [128, 2, 64], F32)
        ls = sb("d_ls", [128, 2], F32)
        le = sb("d_le", [128, 2], F32)
        nlam = sb("d_nlam", [128, 1], F32)
        li = lam_init(layer)
        dl = cx.dlam[:, layer, :]
        P.op("dve", lambda e: e.tensor_tensor(lt[:, 0, :], dl[:, 0:64], dl[:, 64:128], ALU.mult), reads=["consts"], writes=["lt0"])
        P.op("dve", lambda e: e.tensor_tensor(lt[:, 1, :], dl[:, 128:192], dl[:, 192:256], ALU.mult), reads=["consts"], writes=["lt1"])
        P.op("dve", lambda e: e.reduce_sum(ls[:, 0:1], lt[:, 0, :], AX.X), reads=["lt0"], writes=["ls0"])
        P.op("dve", lambda e: e.reduce_sum(ls[:, 1:2], lt[:, 1, :], AX.X), reads=["lt1"], writes=["ls1"])
        P.op("act", lambda e: e.activation(le[:], ls[:], AF.Exp), reads=["ls0", "ls1"], writes=["le"])
        P.op("dve", lambda e: e.tensor_tensor(nlam[:], le[:, 1:2], le[:, 0:1], ALU.subtract), reads=["le"], writes=["nlam"])
        P.op("dve", lambda e: e.tensor_scalar(nlam[:], nlam[:], -li, None, ALU.add), reads=["nlam"], writes=["nlam"])
        SC = 64 ** -0.5
        it = 0
        def do_qt_d(qt):
            nonlocal it
            qs = slice(qt * TT, (qt + 1) * TT)
            nkb = 4 * qt + 4
            PO = (4, 6)
            PR = (5, 7)
            for kb in range(nkb):
                i = it % NB
                it += 1
                o = kb - 4 * qt
                for m in range(2):
                    b = 2 * i + m
                    rows = slice(m * 64, (m + 1) * 64)
                    P.op("pe", lambda e, b=b, kb=kb, rows=rows: e.matmul(
                        ps[b][:], cx.KT[rows, kb * 128:(kb + 1) * 128], cx.QT[rows, qs], start=True, stop=True),
                        writes=[("ps", b)])
                    if o < 0:
                        P.op("act", lambda e, b=b, i=i, m=m: e.activation(pt[m][i][:], ps[b][:], AF.Exp, scale=SC),
                             reads=[("ps", b)], writes=[("pt", m, i)])
                    else:
                        P.op("dve", lambda e, b=b, i=i, m=m, o=o: e.tensor_tensor(tmp[m][i][:], ps[b][:], cx.maskadd[:, o, :], ALU.add),
                             reads=[("ps", b), "consts"], writes=[("tmp", m, i)])
                        P.op("act", lambda e, i=i, m=m: e.activation(pt[m][i][:], tmp[m][i][:], AF.Exp, scale=SC),
                             reads=[("tmp", m, i)], writes=[("pt", m, i)])
                    P.op("pe", lambda e, i=i, m=m, kb=kb: e.matmul(ps[PO[m]][:], cx.V[:, kb, :], pt[m][i][:],
                                                                    start=(kb == 0), stop=(kb == nkb - 1)),
                         reads=[("pt", m, i)], writes=[("ps", PO[m])])
                    P.op("pe", lambda e, i=i, m=m, kb=kb: e.matmul(ps[PR[m]][:], cx.ones_bf[:], pt[m][i][:],
                                                                    start=(kb == 0), stop=(kb == nkb - 1)),
                         reads=[("pt", m, i)], writes=[("ps", PR[m])])
            P.op("dve", lambda e: e.reciprocal(rinv[:], ps[5][:]), reads=[("ps", 5)], writes=["rinv"])
            P.op("dve", lambda e: e.tensor_tensor(oa[:], ps[4][:], rinv[:], ALU.mult), reads=[("ps", 4), "rinv"], writes=["oa"])
            P.op("dve", lambda e: e.reciprocal(rinv[:], ps[7][:]), reads=[("ps", 7), "oa"], writes=["rinv"])
            P.op("dve", lambda e: e.tensor_tensor(ob[:], ps[6][:], rinv[:], ALU.mult), reads=[("ps", 6), "rinv"], writes=["ob"])
            P.op("dve", lambda e: e.scalar_tensor_tensor(oo[:], ob[:], nlam[:], oa[:], ALU.mult, ALU.add),
                 reads=["oa", "ob", "nlam"], writes=["oo"])
            P.op("pool", lambda e: e.tensor_tensor(sq[:], oo[:], oo[:], ALU.mult), reads=["oo"], writes=["sq"])
            P.op("pe", lambda e: e.matmul(ps[4][:], cx.ones_f[:], sq[:], start=True, stop=True), reads=["sq"], writes=[("ps", 4)])
            P.op("act", lambda e: e.activation(lnv[:], ps[4][:], AF.Ln, bias=cx.epsc[:], scale=1.0 / 128), reads=[("ps", 4)], writes=["lnv"])
            P.op("act", lambda e: e.activation(rstd[:], lnv[:], AF.Exp, scale=-0.5), reads=["lnv"], writes=["rstd"])
            P.op("dve", lambda e: e.tensor_tensor(oo[:], oo[:], rstd[:], ALU.mult), reads=["oo", "rstd"], writes=["oo"])
            y = yo[qt % 2]
            P.op("dve", lambda e, y=y: e.tensor_scalar(y[:], oo[:], cx.dgain[:, layer:layer + 1], 1.0 - li, ALU.mult, ALU.mult),
                 reads=["oo", "consts"], writes=[("yo", qt % 2)])
            P.dma("sp", yt_out[128:256, qs], y[:], reads=[("yo", qt % 2)], writes=["yt_out"], chan=("dy", qt % 2))

        for qt in range(NTT):
            do_qt_d(qt)
        P.wait("sp", ["yt_out"])
        P.emit()


CF_IDENT, CF_ONES, CF_MASK, CF_BFG, CF_DGAIN, CF_EPS, CF_PCOEF, CF_PCORR, CF_DLAM = 0, 128, 256, 2304, 2308, 2312, 2313, 2317, 2333
NCF = 2333 + DEPTH * 256


def host_consts(core, inputs):
    cf = np.zeros((128, NCF), np.float32)
    cf[:, CF_IDENT:CF_IDENT + 128] = np.eye(128, dtype=np.float32)
    cf[:, CF_ONES:CF_ONES + 128] = 1.0
    p = np.arange(128)[:, None]
    j = np.arange(TT)[None, :]
    for o in range(4):
        cf[:, CF_MASK + o * TT:CF_MASK + (o + 1) * TT] = np.where(o * 128 + p <= j, 0.0, NEG)
    cf[:, CF_BFG:CF_BFG + DEPTH] = np.asarray(inputs["b_forget"], np.float32)[:, core][None, :]
    cf[:, CF_DGAIN:CF_DGAIN + DEPTH] = np.asarray(inputs["diff_norm_gain"], np.float32).T
    cf[:, CF_EPS] = LN_EPS
    g = core // 2
    w = POOL_W[g]
    cf[:, CF_PCOEF + g] = 1.0 / w
    tt = np.arange(16)
    cf[:, CF_PCORR:CF_PCORR + 16] = (w / np.minimum(tt + 1, w))[None, :]
    cf[:, CF_DLAM:CF_DLAM + DEPTH * 256] = np.asarray(inputs["diff_lambda"], np.float32).reshape(1, DEPTH * 256)
    return cf


def host_rope():
    pos = np.arange(S, dtype=np.float32)
    inv = (np.float32(10000.0) ** (-np.arange(0, 64, 2, dtype=np.float32) / np.float32(64))).astype(np.float32)
    ang = (pos[None, :] * inv[:, None]).astype(np.float32)
    c = np.cos(ang).astype(np.float32)
    s_ = np.sin(ang).astype(np.float32)
    C = np.tile(c, (4, 1))
    Sg = np.concatenate([-s_, s_, -s_, s_], 0)
    return np.ascontiguousarray(C), np.ascontiguousarray(Sg)


def host_w_head(w_in_l, core):
    h = core
    c = lambda off: w_in_l[:, off + h * 128: off + (h + 1) * 128]
    sw = np.concatenate([np.arange(32, 64), np.arange(0, 32), np.arange(96, 128), np.arange(64, 96)])
    fg = np.repeat(w_in_l[:, 13312 + h:13312 + h + 1], 128, axis=1)
    F = np.concatenate([c(0), c(1024), w_in_l[:, 6144 + core * 128:6144 + (core + 1) * 128], fg, c(2048)], 1)
    dq, dk = c(3072), c(4096)
    Dm = np.concatenate([dq, dk, dq[:, sw], dk[:, sw], c(5120)], 1)
    return np.ascontiguousarray(np.stack([F, Dm], 0))


def make_ctx(nc, es, cf_ap, ropeC, ropeS):
    cx = Ctx()
    sb = lambda name, shape, dt: es.enter_context(nc.sbuf_tensor(uname(name), shape, dt))
    cx.ps = [es.enter_context(nc.psum_tensor("ps%d" % i, [128, 512], F32)) for i in range(8)]
    cx.cf = sb("cf_sb", [128, NCF], F32)
    cx.ones_bf = sb("ones_bf", [128, 128], BF16)
    cx.negb = sb("negb", [128, 1], F32)
    cx.ident = cx.cf[:, CF_IDENT:CF_IDENT + 128]
    cx.ones_f = cx.cf[:, CF_ONES:CF_ONES + 128]
    cx.maskadd = cx.cf[:, CF_MASK:CF_MASK + 4 * TT].rearrange("p (o n) -> p o n", o=4)
    cx.bfg = cx.cf[:, CF_BFG:CF_BFG + DEPTH]
    cx.dgain = cx.cf[:, CF_DGAIN:CF_DGAIN + DEPTH]
    cx.epsc = cx.cf[:, CF_EPS:CF_EPS + 1]
    cx.pcoef = cx.cf[:, CF_PCOEF:CF_PCOEF + 4]
    cx.pcorr = cx.cf[:, CF_PCORR:CF_PCORR + 16]
    cx.dlam = cx.cf[:, CF_DLAM:CF_DLAM + DEPTH * 256].rearrange("p (l n) -> p l n", l=DEPTH)
    cx.ropeC, cx.ropeS = ropeC, ropeS
    with ExitStack() as es2:
        P = Prog(nc, es2)
        P.dma("sp", cx.cf[:], cf_ap, writes=["consts"], chan="cf")
        P.op("dve", lambda e: e.memset(cx.ones_bf[:], 1.0), writes=["ones_bf"])
        P.emit()
    return cx


def alloc_attn(nc, es, cx):
    sb = lambda name, shape, dt: es.enter_context(nc.sbuf_tensor(uname(name), shape, dt))
    cx.QT = sb("QT", [128, S], BF16)
    cx.KT = sb("KT", [128, S], BF16)
    cx.V = sb("V", [128, S // 128, 128], BF16)
    cx.Lc = sb("Lc", [128, S], F32)
    cx.ckcol = sb("ckcol", [128, S // 128], F32)


X_GATE = 16 * KC * 3 * 128
X_BR = 16 * 8 * 3 * 128
X_WO = 16 * KC * 128
X_GU = NFF * KC * 2 * 128
NQ = 4
FQ = NFF // NQ
X_DN = NQ * 16 * FQ * 128


LAYERS = list(range(DEPTH))


def seg_list():
    segs = []
    for l in LAYERS:
        segs += [("gate", l, X_GATE), ("br", l, X_BR), ("wo", l, X_WO)]
        if l % 2 == 0:
            segs += [("gu", l, X_GU), ("dn", l, X_DN)]
        else:
            for e in range(NEXP):
                segs += [("gu%d" % e, l, X_GU), ("dn%d" % e, l, X_DN)]
    return segs


def img_gate(w_in_l):
    g = w_in_l[:, 7168:13312].reshape(KC, 128, 3, 16, 128)
    return g.transpose(1, 3, 0, 2, 4).reshape(128, -1)


def img_br(wa, wb, wc):
    w = np.stack([wa, wb, wc], 0).reshape(3, 8, 128, 16, 128)
    return w.transpose(2, 3, 1, 0, 4).reshape(128, -1)


def img_wo(w):
    return w.reshape(KC, 128, 16, 128).transpose(1, 2, 0, 3).reshape(128, -1)


def img_gu(wg, wu):
    w = np.stack([wg, wu], 0).reshape(2, KC, 128, NFF, 128)
    return w.transpose(2, 3, 1, 0, 4).reshape(128, -1)


def img_dn(wd):
    w = wd.reshape(NQ, FQ, 128, 16, 128)
    return w.transpose(2, 0, 3, 1, 4).reshape(128, -1)


def host_wsh(inputs, core):
    r = slice(16 * core, 16 * core + 16)
    parts = []
    f = lambda k: np.asarray(inputs[k], np.float32)
    for (nm, l, X) in seg_list():
        j = l // 2
        if nm == "gate":
            im = img_gate(f("w_in")[l])
        elif nm == "br":
            im = img_br(f("w_branch_a")[l], f("w_branch_b")[l], f("w_branch_c")[l])
        elif nm == "wo":
            im = img_wo(f("w_out")[l])
        elif nm == "gu":
            im = img_gu(f("ffn_w_gate")[j], f("ffn_w_up")[j])
        elif nm == "dn":
            im = img_dn(f("ffn_w_down")[j])
        elif nm.startswith("gu"):
            e = int(nm[2:])
            im = img_gu(f("expert_w_gate")[j, e], f("expert_w_up")[j, e])
        else:
            e = int(nm[2:])
            im = img_dn(f("expert_w_down")[j, e])
        assert im.shape == (128, X), (nm, im.shape, X)
        parts.append(np.ascontiguousarray(im[r]).reshape(-1))
    return np.concatenate(parts)[None, :]


SP_BG, SP_LN, SP_PS, SP_RW, SP_RB = 0, DEPTH * 48, DEPTH * 48 + DEPTH * 64, DEPTH * 48 + DEPTH * 64 + DEPTH * 8, DEPTH * 48 + DEPTH * 64 + DEPTH * 8 + 2 * 128
NSP = SP_RB + 2 * 8


def host_sp32(inputs):
    f = lambda k: np.asarray(inputs[k], np.float32)
    sp = np.zeros((128, NSP), np.float32)
    for l in range(DEPTH):
        sp[:, SP_BG + l * 48:SP_BG + (l + 1) * 48] = f("b_gate")[l].reshape(3, 16, 128).transpose(2, 0, 1).reshape(128, 48)
        for w, k in enumerate(("ln1_g", "ln1_b", "ln2_g", "ln2_b")):
            sp[:, SP_LN + l * 64 + w * 16:SP_LN + l * 64 + (w + 1) * 16] = f(k)[l].reshape(16, 128).T
        sp[:, SP_PS + l * 8:SP_PS + (l + 1) * 8] = f("pool_scale")[l].reshape(8, 128).T
    for j in range(2):
        sp[:, SP_RW + j * 128:SP_RW + (j + 1) * 128] = f("router_w")[j].reshape(KC, 128, 8).transpose(1, 0, 2).reshape(128, 128)
        sp[:, SP_RB + j * 8:SP_RB + (j + 1) * 8] = f("router_b")[j][None, :]
    return sp


def host_wpg(inputs):
    w = np.asarray(inputs["w_pool_group"], np.float32).reshape(DEPTH, 4, 2, 128, 256)
    return np.ascontiguousarray(w.transpose(3, 0, 1, 2, 4).reshape(128, -1))


def host_idxy(core):
    p = np.arange(128)[:, None]
    j = np.arange(24)[None, :]
    br, r = j // 8, j % 8
    return ((r * 384 + br * 128 + p) * 8 + core).astype(np.int32)


def phase_init(nc, cx, wsh, xT_in):
    CH = 8192
    with ExitStack() as es:
        P = Prog(nc, es)
        sb = lambda name, shape, dt: es.enter_context(nc.sbuf_tensor(uname(name), shape, dt))
        stg = [sb("stg", [128, CH], BF16) for _ in range(2)]
        xs = sb("xs", [128, KC, TL], BF16)
        P.dma("pool", xs[:], chunked(xT_in), writes=["xs"], chan="xs")
        P.dma("sp", chunked(cx.xbf_own), xs[:], reads=["xs"], writes=["xbf_own"], chan="xo")
        P.coll("AllGather", ALU.bypass, cx.xbf_own, cx.xT_full, reads=["xbf_own"], writes=["xT_full"], chan="agx")
        off = 0
        k = 0
        for (nm, l, X) in seg_list():
            n = X // 8
            src = wsh[0, off * 16:(off + X) * 16].rearrange("(p n) -> p n", p=128)
            dst = cx.wb[(nm, l)].rearrange("r (a n) -> (r a) n", a=8)
            c0 = 0
            while c0 < n:
                c1 = min(n, c0 + CH)
                i = k % 2
                k += 1
                w = c1 - c0
                assert w % 1024 == 0
                P.dma("pool", stg[i][:, 0:w].rearrange("p (a n) -> p a n", n=1024),
                      src[:, c0:c1].rearrange("p (a n) -> p a n", n=1024),
                      writes=[("stg", i)], chan=("stg", i))
                P.dma("sp", dst[:, c0:c1], stg[i][:, 0:w], reads=[("stg", i)], writes=[("wb", nm, l)], chan=("stgo", i))
                c0 = c1
            if l in cx.defer_layers:
                cx.deferred.append((nm, l))
            else:
                P.coll("AllGather", ALU.bypass, cx.wb[(nm, l)], cx.wf[(nm, l)], reads=[("wb", nm, l)], writes=[("wf", nm, l)], chan="agw")
            off += X
        P.emit()


def need_w(P, cx, nm, layer):
    n = cx.agw_need.get((nm, layer))
    if n is not None:
        P.ext_wait("sp", cx.sem_agw, n)


def phase_b1(nc, cx, layer):
    ps = cx.ps
    with ExitStack() as es:
        P = Prog(nc, es)
        need_w(P, cx, "br", layer)
        sb = lambda name, shape, dt: es.enter_context(nc.sbuf_tensor(uname(name), shape, dt))
        xb = cx.xb
        yy = [sb("b1_y%d" % b, [128, 8, TL], BF16) for b in range(3)]
        yb = sb("b1_yb", [128, 8, TL], BF16)
        hst = [sb("b1_hst", [128, TT], BF16) for _ in range(2)]
        wpg = sb("b1_wpg", [128, 4, 2, 256], BF16)
        wgt = [sb("b1_wg%d" % i, [128, KC, 3, 128], BF16) for i in range(2)]
        wbr = [sb("b1_wb%d" % i, [128, 8, 3, 128], BF16) for i in range(2)]
        sig = [sb("b1_sig%d" % i, [128, 3, TT], BF16) for i in range(2)]
        hm = [sb("b1_hm%d" % i, [128, 3, TT], F32) for i in range(2)]
        idx = sb("b1_idx", [128, 24], mybir.dt.int32)
        import os
        B1S = int(os.environ.get("B1STOP", "9"))
        P.dma("sp", idx[:], cx.idxy, writes=["idx"], chan="idx")
        P.dma("sp", xb[:], chunked(cx.xbf_own), writes=["xb"], chan="xb")
        P.dma("pool", wpg[:], cx.wpg_in[:, layer * 2048:(layer + 1) * 2048].rearrange("p (g k d) -> p g k d", g=4, k=2),
              writes=["wpg"], chan="wpg")
        ytv = cx.yt_full.rearrange("r (tb n) -> (r tb) n", tb=8)
        for j in range(24):
            br, r = j // 8, j % 8
            o = P.op("pool", lambda e, j=j, br=br, r=r: e.indirect_dma_start(
                out=yy[br][:, r, :], out_offset=None, in_=ytv,
                in_offset=bass.IndirectOffsetOnAxis(ap=idx[:, j:j + 1], axis=0)),
                reads=["idx"], writes=[("yy", br, r)])
            o.dma = ("gat", j if os.environ.get("GATC") == "24" else j % 4)
            o.inc = 16
        P.wait("dve", [("yy", br, r) for br in range(3) for r in range(8)])
        k = 0
        for dch in (range(8) if B1S >= 2 else ()):
            g, hf = dch // 2, dch % 2
            for tt in range(2):
                b = k % 2
                k += 1
                ts_ = slice(tt * TT, (tt + 1) * TT)
                for kc in range(2):
                    P.op("pe", lambda e, b=b, g=g, hf=hf, kc=kc, ts_=ts_: e.matmul(
                        ps[b][:], wpg[:, g, kc, hf * 128:(hf + 1) * 128], yy[2][:, g * 2 + kc, ts_], start=(kc == 0), stop=(kc == 1)),
                        reads=["wpg", ("yy", 2, g * 2 + kc)], writes=[("ps", b)])
                if os.environ.get("YBV") == "copy":
                    P.op("dve", lambda e, b=b, dch=dch, ts_=ts_: e.tensor_copy(yb[:, dch, ts_], ps[b][:]),
                         reads=[("ps", b), "consts"], writes=[("yb", dch, tt)])
                else:
                    P.op("dve", lambda e, b=b, dch=dch, ts_=ts_: e.tensor_scalar(
                        yb[:, dch, ts_], ps[b][:], cx.sp32[:, SP_PS + layer * 8 + dch:SP_PS + layer * 8 + dch + 1], None, ALU.mult),
                        reads=[("ps", b), "consts"], writes=[("yb", dch, tt)])
        ysrc = (yy[0], yb, yy[1])
        def ykeys(bi, r, tt):
            if bi == 1:
                return [("yb", r, tt)]
            return [("yy", 0 if bi == 0 else 1, r)]
        wgv = cx.wf[("gate", layer)]
        wbv = cx.wf[("br", layer)]
        CG, CB = KC * 3 * 128, 8 * 3 * 128
        pb = 2
        for dc in (range(16) if B1S >= 3 else ()):
            i = dc % 2
            P.dma("sp", wgt[i][:].rearrange("p k b i -> p (k b i)"), wgv[:, dc * CG:(dc + 1) * CG], writes=[("wgt", i)], chan=("wgt", i))
            P.dma("sp", wbr[i][:].rearrange("p k b i -> p (k b i)"), wbv[:, dc * CB:(dc + 1) * CB], writes=[("wbr", i)], chan=("wbr", i))
            for tt in range(2):
                ts_ = slice(tt * TT, (tt + 1) * TT)
                si = (dc * 2 + tt) % 2
                for bi in range(3):
                    b = 2 + (pb % 6)
                    pb += 1
                    for kc in range(KC):
                        P.op("pe", lambda e, b=b, i=i, kc=kc, bi=bi, ts_=ts_: e.matmul(
                            ps[b][:], wgt[i][:, kc, bi, :], xb[:, kc, ts_], start=(kc == 0), stop=(kc == KC - 1)),
                            reads=[("wgt", i), "xb"], writes=[("ps", b)])
                    bcol = SP_BG + layer * 48 + bi * 16 + dc
                    P.op("act", lambda e, b=b, si=si, bi=bi, bcol=bcol: e.activation(
                        sig[si][:, bi, :], ps[b][:], AF.Sigmoid, bias=cx.sp32[:, bcol:bcol + 1]),
                        reads=[("ps", b), "consts"], writes=[("sig", si, bi)])
                for bi in range(3):
                    b = 2 + (pb % 6)
                    pb += 1
                    for r in range(8):
                        P.op("pe", lambda e, b=b, i=i, r=r, bi=bi, ts_=ts_: e.matmul(
                            ps[b][:], wbr[i][:, r, bi, :], ysrc[bi][:, r, ts_], start=(r == 0), stop=(r == 7)),
                            reads=[("wbr", i)] + ykeys(bi, r, tt), writes=[("ps", b)])
                    P.op("dve", lambda e, b=b, si=si, bi=bi: e.tensor_tensor(hm[si][:, bi, :], ps[b][:], sig[si][:, bi, :], ALU.mult),
                         reads=[("ps", b), ("sig", si, bi)], writes=[("hm", si, bi)])
                P.op("pool", lambda e, si=si: e.tensor_tensor(hm[si][:, 0, :], hm[si][:, 0, :], hm[si][:, 1, :], ALU.add),
                     reads=[("hm", si, 0), ("hm", si, 1)], writes=[("hm", si, 0)])
                P.op("pool", lambda e, si=si: e.tensor_tensor(hst[si][:], hm[si][:, 0, :], hm[si][:, 2, :], ALU.add),
                     reads=[("hm", si, 0), ("hm", si, 2)], writes=[("hst", si)])
                P.dma("sp", cx.hT_d[dc * 128:(dc + 1) * 128, ts_], hst[si][:], reads=[("hst", si)], writes=["hT_d"], chan=("hst", si))
        P.emit()


def emit_ln(P, nc, cx, sbf, x32, xb, gcol, bcol, xkey):
    ps = cx.ps
    sq = [sbf("ln_sq", [128, TT], F32) for _ in range(2)]
    mean = sbf("ln_mean", [128, TT], F32)
    msq = sbf("ln_msq", [128, TT], F32)
    var = sbf("ln_var", [128, TT], F32)
    rstd = sbf("ln_rstd", [128, TT], F32)
    tmp = [sbf("ln_tmp", [128, TT], F32) for _ in range(2)]
    for tt in range(2):
        ts_ = slice(tt * TT, (tt + 1) * TT)
        for dc in range(16):
            i = dc % 2
            P.op("pe", lambda e, dc=dc, ts_=ts_: e.matmul(ps[0][:], cx.ones_f[:], x32[:, dc, ts_], start=(dc == 0), stop=(dc == 15)),
                 reads=[(xkey, dc, tt)], writes=[("ps", 0)])
            P.op("act", lambda e, i=i, dc=dc, ts_=ts_: e.activation(sq[i][:], x32[:, dc, ts_], AF.Square),
                 reads=[(xkey, dc, tt)], writes=[("lnsq", i)])
            P.op("pe", lambda e, i=i, dc=dc: e.matmul(ps[1][:], cx.ones_f[:], sq[i][:], start=(dc == 0), stop=(dc == 15)),
                 reads=[("lnsq", i)], writes=[("ps", 1)])
        P.op("dve", lambda e: e.tensor_scalar(mean[:], ps[0][:], 1.0 / D, None, ALU.mult), reads=[("ps", 0)], writes=["mean"])
        P.op("dve", lambda e: e.tensor_tensor(msq[:], mean[:], mean[:], ALU.mult), reads=["mean"], writes=["msq"])
        P.op("dve", lambda e: e.scalar_tensor_tensor(var[:], ps[1][:], 1.0 / D, msq[:], ALU.mult, ALU.subtract),
             reads=[("ps", 1), "msq"], writes=["var"])
        P.op("act", lambda e: e.activation(var[:], var[:], AF.Ln, bias=cx.epsc[:]), reads=["var"], writes=["var"])
        P.op("act", lambda e: e.activation(rstd[:], var[:], AF.Exp, scale=-0.5), reads=["var"], writes=["rstd"])
        for dc in range(16):
            i = dc % 2
            P.op("dve", lambda e, i=i, dc=dc, ts_=ts_: e.tensor_tensor(tmp[i][:], x32[:, dc, ts_], mean[:], ALU.subtract),
                 reads=[(xkey, dc, tt), "mean"], writes=[("lntmp", i)])
            P.op("pool", lambda e, i=i: e.tensor_tensor(tmp[i][:], tmp[i][:], rstd[:], ALU.mult),
                 reads=[("lntmp", i), "rstd"], writes=[("lntmp", i)])
            P.op("dve", lambda e, i=i, dc=dc, ts_=ts_: e.tensor_scalar(
                x32[:, dc, ts_], tmp[i][:], cx.sp32[:, gcol + dc:gcol + dc + 1], cx.sp32[:, bcol + dc:bcol + dc + 1], ALU.mult, ALU.add),
                reads=[("lntmp", i), "consts"], writes=[(xkey, dc, tt)])
            P.op("act", lambda e, dc=dc, ts_=ts_: e.activation(xb[:, dc, ts_], x32[:, dc, ts_], AF.Copy),
                 reads=[(xkey, dc, tt)], writes=[("xbn", dc, tt)])


def phase_b2(nc, cx, layer, x_res_src):
    ps = cx.ps
    with ExitStack() as es:
        P = Prog(nc, es)
        sbf = lambda name, shape, dt: es.enter_context(nc.sbuf_tensor(uname(name), shape, dt))
        wo = [sbf("b2_wo", [128, KC, 128], BF16) for _ in range(2)]
        need_w(P, cx, "wo", layer)
        x32, xb = cx.x32, cx.xb
        hT = sbf("b2_hT", [128, KC, TL], BF16)
        P.dma("sp", hT[:], chunked(cx.hT_d), writes=["hT"], chan="hTl")
        for q in range(4):
            P.dma("sp", x32[:, q * 4:(q + 1) * 4, :], chunked(x_res_src)[:, q * 4:(q + 1) * 4, :],
                  writes=[("x32", dc, tt) for dc in range(q * 4, q * 4 + 4) for tt in range(2)], chan=("x32l", q))
        wov = cx.wf[("wo", layer)]
        CW = KC * 128
        k = 0
        for dc in range(16):
            i = dc % 2
            P.dma("sp", wo[i][:].rearrange("p k i -> p (k i)"), wov[:, dc * CW:(dc + 1) * CW], writes=[("wo", i)], chan=("wo", i))
            for tt in range(2):
                ts_ = slice(tt * TT, (tt + 1) * TT)
                b = 2 + (k % 4)
                k += 1
                for kc in range(KC):
                    P.op("pe", lambda e, b=b, i=i, kc=kc, ts_=ts_: e.matmul(
                        ps[b][:], wo[i][:, kc, :], hT[:, kc, ts_], start=(kc == 0), stop=(kc == KC - 1)),
                        reads=[("wo", i), "hT"], writes=[("ps", b)])
                P.op("dve", lambda e, b=b, dc=dc, ts_=ts_: e.scalar_tensor_tensor(
                    x32[:, dc, ts_], x32[:, dc, ts_], ALPHA, ps[b][:], ALU.mult, ALU.add),
                    reads=[("ps", b), ("x32", dc, tt)], writes=[("x32", dc, tt)])
        gcol = SP_LN + layer * 64
        emit_ln(P, nc, cx, sbf, x32, xb, gcol, gcol + 16, "x32")
        P.emit()


def phase_b3(nc, cx, layer, x_out, last):
    ps = cx.ps
    moe = (layer % 2 == 1)
    j = layer // 2
    with ExitStack() as es:
        P = Prog(nc, es)
        sbf = lambda name, shape, dt: es.enter_context(nc.sbuf_tensor(uname(name), shape, dt))
        x32, xb = cx.x32, cx.xb
        if layer == 1 and cx.deferred:
            for (nm_, l_) in cx.deferred:
                P.coll_persist("AllGather", ALU.bypass, cx.wb[(nm_, l_)], cx.wf[(nm_, l_)], cx.sem_agw)
                cx.agw_n += 1
                cx.agw_need[(nm_, l_)] = cx.agw_n
            cx.deferred = []
        need_w(P, cx, ("dn%d" % (NEXP - 1)) if moe else "dn", layer)
        act = sbf("b3_act", [128, FQ, TL], BF16)
        wgu = [sbf("b3_wgu", [128, KC, 2, 128], BF16) for _ in range(2)]
        wdn = [sbf("b3_wdn", [128, FQ, 128], BF16) for _ in range(2)]
        sg = [sbf("b3_sg", [128, TT], F32) for _ in range(2)]
        allx = [("x32", dc, tt) for dc in range(16) for tt in range(2)]
        allxb = [("xbn", dc, tt) for dc in range(16) for tt in range(2)]
        if moe:
            lg = sbf("b3_lg", [128, 8, 8], F32)
            mx = sbf("b3_mx", [128, 8, 8], F32)
            nm1 = sbf("b3_nm1", [128, 8], F32)
            msk = sbf("b3_msk", [128, 8, 8], F32)
            ex = sbf("b3_ex", [128, 8, 8], F32)
            ssum = sbf("b3_ss", [128, 8], F32)
            cw = sbf("b3_cw", [128, 8, 8], F32)
            cwb = sbf("b3_cwb", [128, NEXP, TL], F32)
            tmpm = [sbf("b3_tmpm", [128, TT], F32) for _ in range(2)]
            rw = cx.sp32[:, SP_RW + j * 128:SP_RW + (j + 1) * 128].rearrange("p (k e) -> p k e", e=8)
            rb = cx.sp32[:, SP_RB + j * 8:SP_RB + (j + 1) * 8]
            for blk in range(8):
                b = blk % 2
                for kc in range(KC):
                    P.op("pe", lambda e, b=b, kc=kc, blk=blk: e.matmul(
                        ps[b][:, 0:8], x32[:, kc, blk * 128:(blk + 1) * 128], rw[:, kc, :], start=(kc == 0), stop=(kc == KC - 1)),
                        reads=[("x32", kc, blk // 4), "consts"], writes=[("ps", b)])
                P.op("dve", lambda e, b=b, blk=blk: e.tensor_tensor(lg[:, blk, :], ps[b][:, 0:8], rb, ALU.add),
                     reads=[("ps", b), "consts"], writes=[("lg", blk)])
                P.op("dve", lambda e, blk=blk: e.max(mx[:, blk, :], lg[:, blk, :]), reads=[("lg", blk)], writes=[("mx", blk)])
                P.op("dve", lambda e, blk=blk: e.tensor_scalar(msk[:, blk, :], lg[:, blk, :], mx[:, blk, 1:2], None, ALU.is_ge),
                     reads=[("lg", blk), ("mx", blk)], writes=[("msk", blk)])
                P.op("dve", lambda e, blk=blk: e.tensor_scalar(nm1[:, blk:blk + 1], mx[:, blk, 0:1], -1.0, None, ALU.mult),
                     reads=[("mx", blk)], writes=[("nm1", blk)])
                P.op("act", lambda e, blk=blk: e.activation(ex[:, blk, :], lg[:, blk, :], AF.Exp, bias=nm1[:, blk:blk + 1]),
                     reads=[("lg", blk), ("nm1", blk)], writes=[("ex", blk)])
                P.op("dve", lambda e, blk=blk: e.tensor_tensor(ex[:, blk, :], ex[:, blk, :], msk[:, blk, :], ALU.mult),
                     reads=[("ex", blk), ("msk", blk)], writes=[("ex", blk)])
                P.op("dve", lambda e, blk=blk: e.reduce_sum(ssum[:, blk:blk + 1], ex[:, blk, :], AX.X),
                     reads=[("ex", blk)], writes=[("ss", blk)])
                P.op("dve", lambda e, blk=blk: e.reciprocal(ssum[:, blk:blk + 1], ssum[:, blk:blk + 1]),
                     reads=[("ss", blk)], writes=[("ss", blk)])
                P.op("dve", lambda e, blk=blk: e.tensor_scalar(cw[:, blk, :], ex[:, blk, :], ssum[:, blk:blk + 1], None, ALU.mult),
                     reads=[("ex", blk), ("ss", blk)], writes=[("cw", blk)])
                for ee in range(NEXP):
                    b2 = 2 + (ee % 4)
                    P.op("pe", lambda e, b2=b2, blk=blk, ee=ee: e.matmul(
                        ps[b2][:, 0:128], cw[:, blk, ee:ee + 1].to_broadcast([128, 128]), cx.ident, start=True, stop=True),
                        reads=[("cw", blk), "consts"], writes=[("ps", b2)])
                    P.op("act", lambda e, b2=b2, blk=blk, ee=ee: e.activation(cwb[:, ee, blk * 128:(blk + 1) * 128], ps[b2][:, 0:128], AF.Copy),
                         reads=[("ps", b2)], writes=[("cwb", ee, blk // 4)])
        for dc in range(16):
            P.op("pool", lambda e, dc=dc: e.tensor_scalar(x32[:, dc, :], x32[:, dc, :], ALPHA, None, ALU.mult),
                 reads=[("x32", dc, 0), ("x32", dc, 1)], writes=[("x32", dc, 0), ("x32", dc, 1)])
        CGU = KC * 2 * 128
        CDN = FQ * 128
        kgu = 0
        kdn = 0
        pk = 0
        for ex_i in range(NEXP if moe else 1):
            guv = cx.wf[("gu%d" % ex_i if moe else "gu", layer)]
            dnv = cx.wf[("dn%d" % ex_i if moe else "dn", layer)]
            for half in range(NQ):
                for fc in range(FQ):
                    f = half * FQ + fc
                    i = kgu % 2
                    kgu += 1
                    P.dma("sp", wgu[i][:].rearrange("p k g i -> p (k g i)"), guv[:, f * CGU:(f + 1) * CGU], writes=[("wgu", i)], chan=("wgu", i))
                    for tt in range(2):
                        ts_ = slice(tt * TT, (tt + 1) * TT)
                        bg = (pk % 2) * 2
                        bu = bg + 1
                        pk += 1
                        for g_, b in ((0, bg), (1, bu)):
                            for kc in range(KC):
                                P.op("pe", lambda e, b=b, i=i, kc=kc, g_=g_, ts_=ts_: e.matmul(
                                    ps[b][:], wgu[i][:, kc, g_, :], xb[:, kc, ts_], start=(kc == 0), stop=(kc == KC - 1)),
                                    reads=[("wgu", i), ("xbn", kc, tt)], writes=[("ps", b)])
                        si = pk % 2
                        P.op("act", lambda e, bg=bg, si=si: e.activation(sg[si][:], ps[bg][:], AF.Silu),
                             reads=[("ps", bg)], writes=[("sg", si)])
                        P.op("dve", lambda e, bu=bu, si=si, fc=fc, ts_=ts_: e.tensor_tensor(act[:, fc, ts_], ps[bu][:], sg[si][:], ALU.mult),
                             reads=[("ps", bu), ("sg", si)], writes=[("act", fc, tt)])
                for dc in range(16):
                    i = kdn % 2
                    kdn += 1
                    o0 = (half * 16 + dc) * CDN
                    P.dma("sp", wdn[i][:].rearrange("p f i -> p (f i)"), dnv[:, o0:o0 + CDN], writes=[("wdn", i)], chan=("wdn", i))
                    for tt in range(2):
                        ts_ = slice(tt * TT, (tt + 1) * TT)
                        b = 4 + (pk % 4)
                        pk += 1
                        for fc in range(FQ):
                            P.op("pe", lambda e, b=b, i=i, fc=fc, ts_=ts_: e.matmul(
                                ps[b][:], wdn[i][:, fc, :], act[:, fc, ts_], start=(fc == 0), stop=(fc == FQ - 1)),
                                reads=[("wdn", i), ("act", fc, tt)], writes=[("ps", b)])
                        if moe:
                            ti = pk % 2
                            P.op("dve", lambda e, b=b, ti=ti, ex_i=ex_i, ts_=ts_: e.tensor_tensor(tmpm[ti][:], ps[b][:], cwb[:, ex_i, ts_], ALU.mult),
                                 reads=[("ps", b), ("cwb", ex_i, tt)], writes=[("tmpm", ti)])
                            P.op("pool", lambda e, ti=ti, dc=dc, ts_=ts_: e.tensor_tensor(x32[:, dc, ts_], x32[:, dc, ts_], tmpm[ti][:], ALU.add),
                                 reads=[("tmpm", ti), ("x32", dc, tt)], writes=[("x32", dc, tt)])
                        else:
                            P.op("dve", lambda e, b=b, dc=dc, ts_=ts_: e.tensor_tensor(x32[:, dc, ts_], ps[b][:], x32[:, dc, ts_], ALU.add),
                                 reads=[("ps", b), ("x32", dc, tt)], writes=[("x32", dc, tt)])
        P.emit()
    with ExitStack() as es:
        P = Prog(nc, es)
        sbf = lambda name, shape, dt: es.enter_context(nc.sbuf_tensor(uname(name), shape, dt))
        gcol = SP_LN + layer * 64 + 32
        emit_ln(P, nc, cx, sbf, x32, xb, gcol, gcol + 16, "x32")
        if last:
            P.dma("sp", chunked(x_out), x32[:], reads=allx, writes=["x_out"], chan="xout")
        else:
            P.dma("sp", chunked(cx.xres), x32[:], reads=allx, writes=["xres"], chan="xres")
            P.dma("sp", chunked(cx.xbf_own), xb[:], reads=allxb, writes=["xbf_own"], chan="xbo")
            P.coll("AllGather", ALU.bypass, cx.xbf_own, cx.xT_full, reads=["xbf_own"], writes=["xT_full"], chan="agx")
        P.emit()


def build_program(fake_attn=False, stop_after=None):
    nc = bass.Bass("TRN2", target_bir_lowering=False)
    XTOT = sum(X for (_, _, X) in seg_list())
    dt_in = lambda name, shape, dt=F32: nc.dram_tensor(name, shape, dt, kind="ExternalInput").ap()
    xT = dt_in("xT", [D, TL])
    wh = dt_in("wh", [len(LAYERS), 2, D, 640])
    cf = dt_in("cf", [128, NCF])
    rc = dt_in("ropeC", [129, S])[0:128, :]
    rs = dt_in("ropeS", [129, S])[0:128, :]
    wsh = dt_in("wsh", [1, 16 * XTOT])
    sp32 = dt_in("sp32", [128, NSP])
    wpg = dt_in("wpg", [129, DEPTH * 2048])[0:128, :]
    idxy = dt_in("idxy", [128, 24], mybir.dt.int32)
    xo = nc.dram_tensor("xo", [D, TL], F32, kind="ExternalOutput").ap()
    with ExitStack() as es:
        cx = make_ctx(nc, es, cf, rc, rs)
        cx.wpg_in, cx.idxy = wpg, idxy
        cx.xbf_own = nc.dram_tensor("xbf_own", [D, TL], BF16).ap()
        cx.xT_full = nc.dram_tensor("xT_full", [NC * D, TL], BF16).ap()
        cx.yt_own = nc.dram_tensor("yt_own", [384, S], BF16).ap()
        cx.yt_full = nc.dram_tensor("yt_full", [NC * 384, S], BF16).ap()
        cx.xres = nc.dram_tensor("xres", [D, TL], F32).ap()
        cx.hT_d = nc.dram_tensor("hT_d", [D, TL], BF16).ap()
        cx.wb, cx.wf = {}, {}
        for (nm, l, X) in seg_list():
            cx.wb[(nm, l)] = nc.dram_tensor("wb_%s_%d" % (nm, l), [16, X], BF16).ap()
            cx.wf[(nm, l)] = nc.dram_tensor("wf_%s_%d" % (nm, l), [128, X], BF16).ap()
        cx.sem_agw = nc.alloc_semaphore("agw_persist")
        cx.agw_n, cx.agw_need, cx.deferred = 0, {}, []
        cx.defer_layers = (2, 3) if LAYERS == [0, 1, 2, 3] else ()
        cx.sp32 = es.enter_context(nc.sbuf_tensor("sp32_sb", [128, NSP], F32))
        with ExitStack() as es2:
            P = Prog(nc, es2)
            P.dma("sp", cx.sp32[:], sp32, writes=["consts"], chan="sp32")
            P.emit()
        phase_init(nc, cx, wsh, xT)
        if stop_after == "init":
            return nc
        xfull3 = cx.xT_full.rearrange("(r f) n -> r f n", r=NC)
        if fake_attn:
            yt_in = dt_in("yt_in", [384, S], BF16)
        for layer in LAYERS:
            if fake_attn:
                with ExitStack() as es2:
                    P = Prog(nc, es2)
                    tb = es2.enter_context(nc.sbuf_tensor(uname("fk"), [128, 3, S], BF16))
                    P.dma("sp", tb[:], yt_in.rearrange("(a p) n -> p a n", p=128), writes=["tb"], chan="fk1")
                    P.dma("sp", cx.yt_own.rearrange("(a p) n -> p a n", p=128), tb[:], reads=["tb"], writes=["yt_own"], chan="fk2")
                    P.emit()
            else:
              with ExitStack() as es2:
                alloc_attn(nc, es2, cx)
                phase_a1(nc, cx, layer, 'F', xfull3, False, wh[layer - LAYERS[0], 0], cx.yt_own)
                phase_a2_fox(nc, cx, layer, cx.yt_own)
                phase_a1(nc, cx, layer, 'D', xfull3, False, wh[layer - LAYERS[0], 1], cx.yt_own)
                phase_a2_diff(nc, cx, layer, cx.yt_own)
            with ExitStack() as es2:
                P = Prog(nc, es2)
                P.coll("AllGather", ALU.bypass, cx.yt_own, cx.yt_full, writes=["yt_full"], chan="agy")
                P.emit()
            if stop_after == "agy":
                return nc
            with ExitStack() as es2:
                sb = lambda name, shape, dt: es2.enter_context(nc.sbuf_tensor(uname(name), shape, dt))
                cx.xb = sb("xbB", [128, KC, TL], BF16)
                phase_b1(nc, cx, layer)
                if stop_after == "b1":
                    with ExitStack() as es4:
                        P = Prog(nc, es4)
                        tb = es4.enter_context(nc.sbuf_tensor(uname("dbh"), [128, KC, TL], BF16))
                        P.dma("sp", tb[:], chunked(cx.hT_d), writes=["tb"], chan="dbgi")
                        P.dma("sp", chunked(xo.bitcast(BF16)[:, 0:TL]), tb[:], reads=["tb"], writes=["xo"], chan="dbgo")
                        P.emit()
                    return nc
                cx.x32 = sb("x32", [128, KC, TL], F32)
                phase_b2(nc, cx, layer, xT if layer == LAYERS[0] else cx.xres)
                if stop_after == "b2":
                    with ExitStack() as es4:
                        P = Prog(nc, es4)
                        P.dma("sp", chunked(xo), cx.x32[:], writes=["xo"], chan="dbgo")
                        P.emit()
                    return nc
                phase_b3(nc, cx, layer, xo, layer == LAYERS[-1])
    return nc


def tag(a, core):
    return np.concatenate([a, np.full((1, a.shape[1]), core, a.dtype)], 0)


_CACHE = {}
GROUPS = ([0, 1, 2, 3],)


def kernel(**inputs):
    global LAYERS
    f = lambda k: np.asarray(inputs[k], np.float32)
    x = f("x")[0]
    C, Sg = host_rope()
    sp32 = host_sp32(inputs)
    wpg = host_wpg(inputs)
    w_in = f("w_in")
    whs = [np.ascontiguousarray(np.stack([host_w_head(w_in[l], c) for l in range(DEPTH)], 0)) for c in range(NC)]
    cfs = [host_consts(c, inputs) for c in range(NC)]
    for gi, grp in enumerate(GROUPS):
        LAYERS = list(grp)
        if gi not in _CACHE:
            _CACHE[gi] = build_program()
        nc = _CACHE[gi]
        in_maps = []
        for c in range(NC):
            in_maps.append(dict(
                xT=np.ascontiguousarray(x[c * TL:(c + 1) * TL, :].T),
                wh=np.ascontiguousarray(whs[c][LAYERS[0]:LAYERS[-1] + 1]), cf=cfs[c], ropeC=tag(C, c), ropeS=tag(Sg, c),
                wsh=host_wsh(inputs, c), sp32=sp32, wpg=tag(wpg, c), idxy=host_idxy(c)))
        res = run_bass_kernel_spmd(nc, in_maps, core_ids=list(range(NC)))
        del in_maps
        x = np.concatenate([np.asarray(res.results[c]["xo"]).T for c in range(NC)], 0)
    return np.ascontiguousarray(x[None].astype(np.float32))
```
